# Optimizing a Trainium2 kernel written in Bass

```python
import jax, jax.numpy as jnp
from jax import lax
import numpy as np

D_MODEL = 1024
BATCH = 8
SEQ = 8192
DEPTH = 1

HEAD_DIM = 64
MIX_WIDTH = D_MODEL
FOX_WIDTH = MIX_WIDTH // 2
SB_WIDTH = MIX_WIDTH - FOX_WIDTH
N_FOX_HEADS = FOX_WIDTH // HEAD_DIM
N_SB_HEADS = SB_WIDTH // HEAD_DIM
IN_PROJ_WIDTH = 3 * FOX_WIDTH + 3 * SB_WIDTH + N_FOX_HEADS
Q_BLOCK = 128
N_MEM = 256
N_XATTN_HEADS = 4
XATTN_HEAD_DIM = D_MODEL // N_XATTN_HEADS
N_GROUPS = 4
EXPERTS_PER_GROUP = 4
N_EXPERTS = N_GROUPS * EXPERTS_PER_GROUP
TOP_K_IN_GROUP = 2
D_FF_EXPERT = D_MODEL // 4
EPS = 1e-6

kernel_name = "hymba_fox_stickbreak_hmoe_layer"


def _rmsnorm(x, g):
    xf = x.astype(jnp.float32)
    y = xf * lax.rsqrt(jnp.mean(xf * xf, axis=-1, keepdims=True) + EPS)
    return (y * g.astype(jnp.float32)).astype(x.dtype)


def _heads(t, n_heads):
    b, s, _ = t.shape
    return t.reshape(b, s, n_heads, HEAD_DIM).transpose(0, 2, 1, 3)


def _query_blocks(t, n_heads):
    b, s, _ = t.shape
    return t.reshape(b, s // Q_BLOCK, Q_BLOCK, n_heads, HEAD_DIM).transpose(1, 0, 3, 2, 4)


def _merge_blocks(o):
    nb, b, h, qb, dh = o.shape
    return o.transpose(1, 0, 3, 2, 4).reshape(b, nb * qb, h * dh)


def _forgetting_attention(q, k, v, f_logit):
    b, s, _ = q.shape
    nb = s // Q_BLOCK
    c = jnp.cumsum(jax.nn.log_sigmoid(f_logit.astype(jnp.float32)), axis=1).transpose(0, 2, 1)
    c_blocks = c.reshape(b, N_FOX_HEADS, nb, Q_BLOCK).transpose(2, 0, 1, 3)
    qb = _query_blocks(q, N_FOX_HEADS)
    kh = _heads(k, N_FOX_HEADS)
    vh = _heads(v, N_FOX_HEADS)
    starts = jnp.arange(nb, dtype=jnp.int32) * Q_BLOCK
    kpos = jnp.arange(s, dtype=jnp.int32)
    scale = HEAD_DIM ** -0.5

    def one_block(args):
        q_blk, c_blk, start = args
        qpos = start + jnp.arange(Q_BLOCK, dtype=jnp.int32)
        logits = jnp.einsum('bhqd,bhkd->bhqk', q_blk, kh).astype(jnp.float32) * scale
        logits = logits + c_blk[..., None] - c[:, :, None, :]
        logits = jnp.where(kpos[None, :] <= qpos[:, None], logits, -jnp.inf)
        p = jax.nn.softmax(logits, axis=-1)
        return jnp.einsum('bhqk,bhkd->bhqd', p.astype(vh.dtype), vh)

    return _merge_blocks(lax.map(one_block, (qb, c_blocks, starts)))


def _stick_breaking_attention(q, k, v):
    b, s, _ = q.shape
    nb = s // Q_BLOCK
    qb = _query_blocks(q, N_SB_HEADS)
    kh = _heads(k, N_SB_HEADS)
    vh = _heads(v, N_SB_HEADS)
    starts = jnp.arange(nb, dtype=jnp.int32) * Q_BLOCK
    kpos = jnp.arange(s, dtype=jnp.int32)
    scale = HEAD_DIM ** -0.5

    def one_block(args):
        q_blk, start = args
        qpos = start + jnp.arange(Q_BLOCK, dtype=jnp.int32)
        mask = kpos[None, :] < qpos[:, None]
        z = jnp.einsum('bhqd,bhkd->bhqk', q_blk, kh).astype(jnp.float32) * scale
        log_one_minus = jnp.where(mask, jax.nn.log_sigmoid(-z), 0.0)
        tail = lax.cumsum(log_one_minus, axis=3, reverse=True) - log_one_minus
        weight = jnp.where(mask, jnp.exp(jax.nn.log_sigmoid(z) + tail), 0.0)
        return jnp.einsum('bhqk,bhkd->bhqd', weight.astype(vh.dtype), vh)

    return _merge_blocks(lax.map(one_block, (qb, starts)))


def _memory_cross_attention(hn, mn, w_xq, w_xkv, w_xo):
    b, s, d = hn.shape
    m = mn.shape[1]
    q = (hn @ w_xq).reshape(b, s, N_XATTN_HEADS, XATTN_HEAD_DIM)
    kv = mn @ w_xkv
    k = kv[..., :d].reshape(b, m, N_XATTN_HEADS, XATTN_HEAD_DIM)
    v = kv[..., d:].reshape(b, m, N_XATTN_HEADS, XATTN_HEAD_DIM)
    logits = jnp.einsum('bshd,bmhd->bhsm', q, k).astype(jnp.float32) * (XATTN_HEAD_DIM ** -0.5)
    p = jax.nn.softmax(logits, axis=-1)
    o = jnp.einsum('bhsm,bmhd->bshd', p.astype(v.dtype), v).reshape(b, s, d)
    return o @ w_xo


def _hierarchical_moe(u, w_rg, b_rg, w_re, b_re, w_gate, w_up, w_down):
    b, s, d = u.shape
    t = u.reshape(b * s, d)
    tf = t.astype(jnp.float32)
    p_group = jax.nn.softmax(tf @ w_rg.astype(jnp.float32) + b_rg.astype(jnp.float32), axis=-1)
    g_val, g_idx = lax.top_k(p_group, 1)
    g_val, g_idx = g_val[:, 0], g_idx[:, 0]
    e_logits = (tf @ w_re.astype(jnp.float32) + b_re.astype(jnp.float32)).reshape(-1, N_GROUPS, EXPERTS_PER_GROUP)
    in_group = jnp.take_along_axis(e_logits, g_idx[:, None, None], axis=1)[:, 0, :]
    e_val, e_idx = lax.top_k(in_group, TOP_K_IN_GROUP)
    weights = g_val[:, None] * jax.nn.softmax(e_val, axis=-1)
    expert_id = g_idx[:, None] * EXPERTS_PER_GROUP + e_idx
    gate = jnp.sum(jax.nn.one_hot(expert_id, N_EXPERTS, dtype=jnp.float32) * weights[..., None], axis=1)
    y = jnp.zeros((b * s, d), jnp.float32)
    for e in range(N_EXPERTS):
        hidden = jax.nn.silu(t @ w_gate[e]) * (t @ w_up[e])
        y = y + gate[:, e:e + 1] * (hidden @ w_down[e]).astype(jnp.float32)
    return y.astype(u.dtype).reshape(b, s, d)


def setup_inputs(seed: int = 0) -> dict:
    key = jax.random.key(seed)
    ks = jax.random.split(key, 24)
    f32 = jnp.float32
    nrm = lambda k, shape, scale: jax.random.normal(k, shape, f32) * scale
    gain = lambda k, shape: 1.0 + 0.02 * jax.random.normal(k, shape, f32)
    L = DEPTH
    return {
        "x": jax.random.normal(ks[0], (BATCH, SEQ, D_MODEL), f32),
        "mem": jax.random.normal(ks[1], (BATCH, N_MEM, D_MODEL), f32),
        "norm_mix": gain(ks[2], (L, D_MODEL)),
        "w_in": nrm(ks[3], (L, D_MODEL, IN_PROJ_WIDTH), D_MODEL ** -0.5),
        "b_forget": 3.0 + 0.5 * jax.random.normal(ks[4], (L, N_FOX_HEADS), f32),
        "norm_fox_out": gain(ks[5], (L, FOX_WIDTH)),
        "norm_sb_out": gain(ks[6], (L, SB_WIDTH)),
        "w_out": nrm(ks[7], (L, MIX_WIDTH, D_MODEL), MIX_WIDTH ** -0.5),
        "norm_xattn": gain(ks[8], (L, D_MODEL)),
        "norm_mem": gain(ks[9], (L, D_MODEL)),
        "w_xq": nrm(ks[10], (L, D_MODEL, D_MODEL), D_MODEL ** -0.5),
        "w_xkv": nrm(ks[11], (L, D_MODEL, 2 * D_MODEL), D_MODEL ** -0.5),
        "w_xo": nrm(ks[12], (L, D_MODEL, D_MODEL), D_MODEL ** -0.5),
        "norm_ffn": gain(ks[13], (L, D_MODEL)),
        "w_router_group": nrm(ks[14], (L, D_MODEL, N_GROUPS), D_MODEL ** -0.5),
        "b_router_group": nrm(ks[15], (L, N_GROUPS), 0.01),
        "w_router_expert": nrm(ks[16], (L, D_MODEL, N_EXPERTS), D_MODEL ** -0.5),
        "b_router_expert": nrm(ks[17], (L, N_EXPERTS), 0.01),
        "w_exp_gate": nrm(ks[18], (L, N_EXPERTS, D_MODEL, D_FF_EXPERT), D_MODEL ** -0.5),
        "w_exp_up": nrm(ks[19], (L, N_EXPERTS, D_MODEL, D_FF_EXPERT), D_MODEL ** -0.5),
        "w_exp_down": nrm(ks[20], (L, N_EXPERTS, D_FF_EXPERT, D_MODEL), D_FF_EXPERT ** -0.5),
        "norm_final": gain(ks[21], (D_MODEL,)),
    }


def reference(x, mem, norm_mix, w_in, b_forget, norm_fox_out, norm_sb_out, w_out,
              norm_xattn, norm_mem, w_xq, w_xkv, w_xo, norm_ffn,
              w_router_group, b_router_group, w_router_expert, b_router_expert,
              w_exp_gate, w_exp_up, w_exp_down, norm_final):
    h = x
    F, S_ = FOX_WIDTH, SB_WIDTH
    for l in range(DEPTH):
        u = _rmsnorm(h, norm_mix[l])
        proj = u @ w_in[l]
        q_a = proj[..., 0:F]
        k_a = proj[..., F:2 * F]
        v_a = proj[..., 2 * F:3 * F]
        o0 = 3 * F
        q_b = proj[..., o0:o0 + S_]
        k_b = proj[..., o0 + S_:o0 + 2 * S_]
        v_b = proj[..., o0 + 2 * S_:o0 + 3 * S_]
        f_logit = proj[..., o0 + 3 * S_:] + b_forget[l]
        o_a = _forgetting_attention(q_a, k_a, v_a, f_logit)
        o_b = _stick_breaking_attention(q_b, k_b, v_b)
        mixed = jnp.concatenate([_rmsnorm(o_a, norm_fox_out[l]), _rmsnorm(o_b, norm_sb_out[l])], axis=-1)
        h = h + (mixed @ w_out[l]).astype(h.dtype)
        h = h + _memory_cross_attention(_rmsnorm(h, norm_xattn[l]), _rmsnorm(mem, norm_mem[l]),
                                        w_xq[l], w_xkv[l], w_xo[l]).astype(h.dtype)
        h = h + _hierarchical_moe(_rmsnorm(h, norm_ffn[l]), w_router_group[l], b_router_group[l],
                                  w_router_expert[l], b_router_expert[l],
                                  w_exp_gate[l], w_exp_up[l], w_exp_down[l]).astype(h.dtype)
    return _rmsnorm(h, norm_final)
```

```python
import numpy as np
from contextlib import ExitStack
import concourse.bass as bass
import concourse.mybir as mybir
from concourse.bass_utils import run_bass_kernel_spmd

F32 = mybir.dt.float32
BF16 = mybir.dt.bfloat16
AF = mybir.ActivationFunctionType
ALU = mybir.AluOpType
AX = mybir.AxisListType

D = 1024
NMEM = 256
NEXP = 16
DFF = 256
EPS = 1e-6
WIN = 3080
BIG = 1.0e30
NEG = -30000.0
SEM_ROT = 30000
FORCE_SB_FULL = False


class Tok:
    __slots__ = ("sem", "val", "eng")

    def __init__(self, sem, val, eng):
        self.sem, self.val, self.eng = sem, val, eng


class Buf:
    __slots__ = ("name", "w", "r")

    def __init__(self, name):
        self.name, self.w, self.r = name, None, {}


class Prog:
    ENGS = ("pe", "act", "dve", "pool", "sp")

    def __init__(self, nc, es):
        self.nc, self.es = nc, es
        self.q = {e: [] for e in self.ENGS}
        self.sem = {}
        self.cnt = {}
        self.seen = {e: {} for e in self.ENGS}
        self.nsem = 0
        for e in ("pe", "act", "dve", "pool"):
            self._rot(e)
        self.dsem = {}
        self.all_dma_toks = []
        self.region = None
        self.reg_consts = {e: [] for e in self.ENGS}
        self.regs = {}

    def _new_sem(self, name):
        self.nsem += 1
        return self.es.enter_context(self.nc.semaphore(f"{name}_{self.nsem}"))

    def _rot(self, e):
        self.sem[e] = self._new_sem("s" + e)
        self.cnt[e] = 0

    def _waits(self, eng, reads, writes, extra):
        deps = []
        for b in reads:
            if b.w is not None:
                deps.append(b.w)
        for b in writes:
            if b.w is not None:
                deps.append(b.w)
            deps.extend(b.r.values())
        deps.extend(extra)
        best = {}
        for t in deps:
            if t is None:
                continue
            if t.eng == eng and eng == "pe":
                continue
            k = id(t.sem)
            if self.seen[eng].get(k, 0) >= t.val:
                continue
            if k not in best or best[k].val < t.val:
                best[k] = t
        out = []
        for k, t in best.items():
            self.seen[eng][k] = t.val
            out.append((t.sem, t.val))
        return out

    def _commit(self, tok, reads, writes):
        for b in writes:
            b.w = tok
            b.r = {}
        for b in reads:
            if b not in writes:
                k = id(tok.sem)
                if k not in b.r or b.r[k].val < tok.val:
                    b.r[k] = tok

    def region_begin(self, engs, flag_ap, flag_bufs):
        assert self.region is None
        for e in engs:
            if self.cnt[e] > SEM_ROT - 6000:
                self._rot(e)
            waits = self._waits(e, flag_bufs, (), ())
            self.q[e].append(("IF", flag_ap, waits))
        self.region = dict(engs=tuple(engs), cnt={e: self.cnt[e] for e in engs},
                           seen={e: dict(self.seen[e]) for e in engs}, dsem={e: {} for e in engs})

    def region_end(self):
        r = self.region
        for e in r["engs"]:
            self.q[e].append(("ELSE", self.cnt[e] - r["cnt"][e], self.sem[e], list(r["dsem"][e].values())))
            self.seen[e] = r["seen"][e]
        self.region = None

    def op(self, eng, fn, reads=(), writes=(), extra=()):
        if self.region is not None:
            assert eng in self.region["engs"], f"{eng} op inside region"
        waits = self._waits(eng, reads, writes, extra)
        if self.cnt[eng] >= SEM_ROT:
            assert self.region is None
            self._rot(eng)
        self.cnt[eng] += 1
        tok = Tok(self.sem[eng], self.cnt[eng], eng)
        self.q[eng].append((fn, waits, (tok.sem, 1)))
        self._commit(tok, reads, writes)
        return tok

    def dma(self, queue, key, out, in_, reads=(), writes=(), extra=(), **kw):
        return self.dma_fn(queue, key, lambda e: e.dma_start(out=out, in_=in_, **kw), reads, writes, extra)

    def dma_fn(self, queue, key, fn, reads=(), writes=(), extra=()):
        if self.region is not None:
            assert queue in self.region["engs"], f"{queue} dma inside region"
        waits = self._waits(queue, reads, writes, extra)
        if key not in self.dsem:
            self.dsem[key] = [self._new_sem("d"), 0]
        ds = self.dsem[key]
        ds[1] += 16
        tok = Tok(ds[0], ds[1], "dma")
        self.q[queue].append((fn, waits, (tok.sem, 16)))
        if self.region is not None:
            d = self.region["dsem"][queue]
            if id(ds[0]) not in d:
                d[id(ds[0])] = [ds[0], 0]
            d[id(ds[0])][1] += 16
        self._commit(tok, reads, writes)
        self.all_dma_toks.append(tok)
        return tok

    def barrier(self):
        toks = [Tok(self.sem[e], self.cnt[e], e) for e in ("pe", "act", "dve", "pool") if self.cnt[e] > 0]
        last = {}
        for t in self.all_dma_toks:
            k = id(t.sem)
            if k not in last or last[k].val < t.val:
                last[k] = t
        toks += list(last.values())
        for e in self.ENGS:
            ws = []
            for t in toks:
                if t.eng == e:
                    continue
                k = id(t.sem)
                if self.seen[e].get(k, 0) >= t.val:
                    continue
                self.seen[e][k] = t.val
                ws.append((t.sem, t.val))
            if ws:
                self.q[e].append((None, ws, None))

    def emit(self):
        nc = self.nc
        with nc.Block() as block:
            def run(e, lst, name):
                reg = None
                guard = None
                for nm, val in self.reg_consts[name]:
                    r_ = e.alloc_register(nm)
                    e.reg_mov(r_, val)
                    self.regs[nm] = r_
                for item in lst:
                    if isinstance(item[0], str):
                        if item[0] == "IF":
                            if reg is None:
                                reg = e.alloc_register("flag_" + name)
                            for s, v in item[2]:
                                e.wait_ge(s, v)
                            e.reg_load(reg, item[1])
                            guard = e.If_ne(reg, 0)
                            guard.__enter__()
                        else:
                            guard.__exit__(None, None, None)
                            g2 = e.Else()
                            g2.__enter__()
                            n_ = item[1]
                            while n_ > 0:
                                e.sem_inc(item[2], min(n_, 240))
                                n_ -= 240
                            for ds_, n_ in item[3]:
                                while n_ > 0:
                                    e.sem_inc(ds_, min(n_, 240))
                                    n_ -= 240
                            if item[1] <= 0 and not item[3]:
                                e.nop()
                            g2.__exit__(None, None, None)
                        continue
                    fn, waits, inc = item
                    for s, v in waits:
                        e.wait_ge(s, v)
                    if fn is not None:
                        ins = fn(e)
                        if inc is not None:
                            ins.then_inc(inc[0], inc[1])

            @block.tensor
            def _(e):
                run(e, self.q["pe"], "pe")

            @block.scalar
            def _(e):
                run(e, self.q["act"], "act")

            @block.vector
            def _(e):
                run(e, self.q["dve"], "dve")

            @block.gpsimd
            def _(e):
                run(e, self.q["pool"], "pool")

            @block.sync
            def _(e):
                run(e, self.q["sp"], "sp")


class Arena:
    def __init__(self, ap, total):
        self.ap, self.total, self.top = ap, total, 0

    def alloc(self, n, dtype=BF16):
        n16 = n * 2 if dtype == F32 else n
        self.top = (self.top + 31) // 32 * 32
        off = self.top
        self.top += n16
        assert self.top <= self.total, f"arena overflow {self.top} > {self.total}"
        v = self.ap[:, off:off + n16]
        if dtype == F32:
            v = v.bitcast(F32)
        return v

    def mark(self):
        return self.top

    def release(self, m):
        self.top = m


def build_program(S, debug=False):
    assert S % 1024 == 0
    NB = S // 128
    NCH = S // 512
    NT3 = S // 512
    ROWW = 1032
    CAPG = S + 512
    NROWS = 4 * CAPG
    I32 = mybir.dt.int32
    nc = bass.Bass("TRN2", target_bir_lowering=False)

    def din(name, shape):
        return nc.dram_tensor(name, shape, F32, kind="ExternalInput").ap()

    x_d = din("x", [S, D])
    mem_d = din("mem", [NMEM, D])
    g_mix_d = din("norm_mix", [D])
    w_in_d = din("w_in", [D, WIN])
    b_forget_d = din("b_forget", [8])
    g_fox_d = din("norm_fox_out", [512])
    g_sb_d = din("norm_sb_out", [512])
    w_out_d = din("w_out", [D, D])
    g_x_d = din("norm_xattn", [D])
    g_mem_d = din("norm_mem", [D])
    w_xq_d = din("w_xq", [D, D])
    w_xkv_d = din("w_xkv", [D, 2 * D])
    w_xo_d = din("w_xo", [D, D])
    g_f_d = din("norm_ffn", [D])
    w_rg_d = din("w_router_group", [D, 4])
    b_rg_d = din("b_router_group", [4])
    w_re_d = din("w_router_expert", [D, 16])
    b_re_d = din("b_router_expert", [16])
    wg_d = din("w_exp_gate", [NEXP, D, DFF])
    wu_d = din("w_exp_up", [NEXP, D, DFF])
    wd_d = din("w_exp_down", [NEXP, DFF, D])
    g_fin_d = din("norm_final", [D])
    out_d = nc.dram_tensor("out", [S, D], F32, kind="ExternalOutput").ap()

    skind = "ExternalOutput" if debug else "Internal"

    def dscr(name, shape, dt):
        return nc.dram_tensor(name, shape, dt, kind=skind).ap()

    w_in_b = dscr("w_in_b", [D, WIN], BF16)
    w_out_b = dscr("w_out_b", [D, D], BF16)
    w_xq_b = dscr("w_xq_b", [D, D], BF16)
    w_xkv_b = dscr("w_xkv_b", [D, 2 * D], BF16)
    w_xo_b = dscr("w_xo_b", [D, D], BF16)
    wg_b = dscr("wg_b", [NEXP, D, DFF], BF16)
    wu_b = dscr("wu_b", [NEXP, D, DFF], BF16)
    wd_b = dscr("wd_b", [NEXP, DFF, D], BF16)
    qkT_d = dscr("qkT_s", [2048, S], BF16)
    vs_d = dscr("vs_s", [S, D], BF16)
    os_d = dscr("os_s", [S, D], F32)
    sorted_d = dscr("sorted_s", [NROWS, ROWW], F32)
    if debug:
        dbg_lg = dscr("dbg_lg", [S // 1024, 128, 8 * 20], F32)
        dbg_oh = dscr("dbg_oh", [S // 1024, 128, 8 * 4], F32)
        dbg_h = dscr("dbg_h", [S // 1024, 128, 8 * 1032], F32)

    es = ExitStack()
    with es:
        ARENA_N = 103 * 1024 + 896
        arena_t = es.enter_context(nc.sbuf_tensor("arena", [128, ARENA_N], BF16))
        ps_t = es.enter_context(nc.psum_tensor("ps", [128, 4096], F32))
        P = Prog(nc, es)
        P.reg_consts["pool"] += [("bc_sorted", NROWS - 1), ("bc_out", S - 1)]
        A = Arena(arena_t[:], ARENA_N)

        def bank(i, n=1):
            return ps_t[:, i * 512:(i + n) * 512]

        bankB = [Buf(f"bank{i}") for i in range(8)]

        ident_bf = A.alloc(128)
        ident_f = A.alloc(128, F32)
        negtri = A.alloc(128)
        negones = A.alloc(128)
        ones_bf = A.alloc(128)
        mask_fox = A.alloc(128)
        mask_sb = A.alloc(128)
        m01_sb = A.alloc(128)
        tri_f = A.alloc(128, F32)
        ones_f = A.alloc(128, F32)
        zeros_bf = A.alloc(512)
        sel0 = A.alloc(128)
        sel1 = A.alloc(128)
        g_cat = A.alloc(D, F32)
        g_x = A.alloc(D, F32)
        g_mem = A.alloc(D, F32)
        g_f = A.alloc(D, F32)
        g_fin = A.alloc(D, F32)
        bfor = A.alloc(8, F32)
        rbias = A.alloc(8 * 20, F32)
        NST = 8
        st_ss = [A.alloc(1, F32) for _ in range(NST)]
        junk = A.alloc(D)
        NBI = max(NB, 8)
        NCI = NBI + NT3 + 4
        c_i = A.alloc(NCI, F32).bitcast(I32)
        c_f = A.alloc(NCI, F32)
        iota_f = c_f[:, 0:NBI]
        thr512 = c_f[:, NBI:NBI + NT3]
        goff = c_f[:, NBI + NT3:NCI]
        w8421 = A.alloc(8 * 4, F32)
        cnt_run = A.alloc(4, F32)
        flags3 = A.alloc(4 * NT3, F32).bitcast(I32)
        padrow = A.alloc(ROWW, F32)
        mark0 = A.mark()
        g_mix = A.alloc(D, F32)
        fl = A.alloc(NB * 8, F32)
        a_full = A.alloc(NB * 8, F32)
        off = A.alloc(NB * 8, F32)
        CONST = Buf("const")
        FL = Buf("fl")
        AFULL = Buf("afull")
        OFFB = Buf("off")

        def cfill(ap, val, cmp=None, sign=1, fill=0.0):
            P.op("pool", lambda e: e.memset(ap, val), writes=[CONST])
            if cmp is not None:
                P.op("pool", lambda e: e.affine_select(out=ap, in_=ap, pattern=[[-sign, 128]], compare_op=cmp, fill=fill,
                                                       base=0, channel_multiplier=sign), reads=[CONST], writes=[CONST])

        cfill(ident_bf, 0.0, ALU.not_equal, fill=1.0)
        cfill(ident_f, 0.0, ALU.not_equal, fill=1.0)
        cfill(negtri, -1.0, ALU.is_ge)
        cfill(negones, -1.0)
        cfill(ones_bf, 1.0)
        cfill(mask_fox, NEG, ALU.is_gt)
        cfill(mask_sb, NEG, ALU.is_ge)
        cfill(m01_sb, 1.0, ALU.is_gt, sign=-1)
        cfill(tri_f, 1.0, ALU.is_ge, sign=-1)
        cfill(ones_f, 1.0)
        cfill(zeros_bf, 0.0)
        P.op("pool", lambda e: e.memset(sel0[0:64, :], 1.0), writes=[CONST])
        P.op("pool", lambda e: e.memset(sel0[64:128, :], 0.0), writes=[CONST])
        P.op("pool", lambda e: e.memset(sel1[0:64, :], 0.0), writes=[CONST])
        P.op("pool", lambda e: e.memset(sel1[64:128, :], 1.0), writes=[CONST])

        CONST2 = Buf("const2")
        P.op("pool", lambda e: e.iota(out=c_i[:, 0:NBI], pattern=[[128, NBI]], base=0, channel_multiplier=1), writes=[CONST])
        P.op("pool", lambda e: e.iota(out=c_i[:, NBI:NBI + NT3], pattern=[[512, NT3]], base=0, channel_multiplier=0), writes=[CONST])
        P.op("pool", lambda e: e.iota(out=c_i[:, NBI + NT3:NCI], pattern=[[CAPG, 4]], base=0, channel_multiplier=0), writes=[CONST])
        P.op("dve", lambda e: e.tensor_copy(out=c_f, in_=c_i), reads=[CONST], writes=[CONST2])
        w8421_3 = w8421.rearrange("p (g n) -> p g n", n=4)
        for i_ in range(4):
            P.op("pool", lambda e, i_=i_: e.memset(w8421_3[:, :, i_:i_ + 1], float(2 ** (3 - i_))), writes=[CONST])
        P.op("pool", lambda e: e.memset(padrow, 0.0), writes=[CONST])
        P.op("pool", lambda e: e.memset(padrow[:, 1028:1029], 1.0e6), reads=[CONST], writes=[CONST])

        GC = Buf("gconst")
        P.dma("sp", "c0", g_mix, g_mix_d.partition_broadcast(128), writes=[GC])
        P.dma("sp", "c0", g_cat[:, 0:512], g_fox_d.partition_broadcast(128), writes=[GC])
        P.dma("sp", "c0", g_cat[:, 512:1024], g_sb_d.partition_broadcast(128), writes=[GC])
        P.dma("sp", "c0", g_x, g_x_d.partition_broadcast(128), writes=[GC])
        P.dma("sp", "c0", g_mem, g_mem_d.partition_broadcast(128), writes=[GC])
        P.dma("sp", "c0", g_f, g_f_d.partition_broadcast(128), writes=[GC])
        P.dma("sp", "c0", g_fin, g_fin_d.partition_broadcast(128), writes=[GC])
        P.dma("sp", "c0", bfor, b_forget_d.partition_broadcast(128), writes=[GC])
        rb3 = rbias.rearrange("p (g n) -> p g n", n=20)
        for gi in range(8):
            P.dma("sp", "c0", rb3[:, gi, 0:4], b_rg_d.partition_broadcast(128), writes=[GC])
            P.dma("sp", "c0", rb3[:, gi, 4:20], b_re_d.partition_broadcast(128), writes=[GC])

        WB = {k: Buf("wb_" + k) for k in ("in", "out", "xq", "xkv", "xo")}
        P.dma("pool", "cv_in", w_in_b, w_in_d, writes=[WB["in"]], max_dma_last_dim=4096)
        WGB, WUB, WDB = Buf("wgb"), Buf("wub"), Buf("wdb")

        def late_conversions():
            P.dma("pool", "cv_out", w_out_b, w_out_d, writes=[WB["out"]], max_dma_last_dim=4096)
            P.dma("pool", "cv_xq", w_xq_b, w_xq_d, writes=[WB["xq"]], max_dma_last_dim=4096)
            P.dma("pool", "cv_xkv", w_xkv_b, w_xkv_d, writes=[WB["xkv"]], max_dma_last_dim=4096)
            P.dma("pool", "cv_xo", w_xo_b, w_xo_d, writes=[WB["xo"]], max_dma_last_dim=4096)
            for q4 in range(4):
                P.dma("pool", "cv_g", wg_b[q4 * 4:(q4 + 1) * 4], wg_d[q4 * 4:(q4 + 1) * 4], writes=[WGB], max_dma_last_dim=4096)
                P.dma("pool", "cv_u", wu_b[q4 * 4:(q4 + 1) * 4], wu_d[q4 * 4:(q4 + 1) * 4], writes=[WUB], max_dma_last_dim=4096)
                P.dma("pool", "cv_d", wd_b[q4 * 4:(q4 + 1) * 4], wd_d[q4 * 4:(q4 + 1) * 4], writes=[WDB], max_dma_last_dim=4096)

        QK = Buf("qkT")
        VS = Buf("vs")
        OS = Buf("os")

        st_B = [Buf(f"st{i}") for i in range(NST)]
        JUNK = Buf("junk")
        st_ctr = [0]

        def rms_scale(src_ap, src_bufs, width, extra_reads=()):
            k = st_ctr[0] % NST
            st_ctr[0] += 1
            ss, sb_ = st_ss[k], st_B[k]
            P.op("pool", lambda e: e.memset(ss, 0.0), writes=[sb_])
            P.op("act", lambda e: e.activation(out=junk[:, 0:width], in_=src_ap, func=AF.Square, accum_out=ss),
                 reads=list(src_bufs) + list(extra_reads), writes=[JUNK, sb_])
            P.op("act", lambda e: e.activation(out=ss, in_=ss, func=AF.Ln, scale=1.0 / width, bias=EPS),
                 reads=[sb_], writes=[sb_])
            P.op("act", lambda e: e.activation(out=ss, in_=ss, func=AF.Exp, scale=-0.5), reads=[sb_], writes=[sb_])
            return ss, sb_

        def transposes_bf(src_ap, src_bufs, bnk, dst_ap3, dst_bufs, nchunks=8, copy_eng="act"):
            pb = bank(bnk).bitcast(BF16)

            def f(e):
                for c in range(nchunks):
                    ins = e.transpose(out=pb[:, c * 128:(c + 1) * 128], in_=src_ap[:, c * 128:(c + 1) * 128],
                                      identity=ident_bf)
                return ins
            P.op("pe", f, reads=list(src_bufs) + [CONST], writes=[bankB[bnk]])
            src3 = pb[:, 0:nchunks * 128].rearrange("p (c t) -> p c t", t=128)
            if copy_eng == "act":
                P.op("act", lambda e: e.copy(out=dst_ap3, in_=src3), reads=[bankB[bnk]], writes=list(dst_bufs))
            else:
                P.op("dve", lambda e: e.tensor_copy(out=dst_ap3, in_=src3), reads=[bankB[bnk]], writes=list(dst_bufs))

        base_mark = A.mark()

        w_in_sb = A.alloc(8 * WIN).rearrange("p (c n) -> p c n", n=WIN)
        WINSB = Buf("w_in_sb")
        P.dma("sp", "w_in_ld", w_in_sb, w_in_b.rearrange("(c p) n -> p c n", p=128), reads=[WB["in"]], writes=[WINSB])
        xin = [A.alloc(4 * D, F32).rearrange("p (b d) -> p b d", d=D) for _ in range(2)]
        XIN = [[Buf(f"xin{i}_{b}") for b in range(4)] for i in range(2)]
        ub = [A.alloc(D) for _ in range(2)]
        UB = [Buf(f"ub{i}") for i in range(2)]
        uT = [A.alloc(8 * 512).rearrange("p (c t) -> p c t", t=512) for _ in range(2)]
        UT = [[Buf(f"uT{i}_{b}") for b in range(4)] for i in range(2)]
        qk_st = [A.alloc(512) for _ in range(4)]
        QKST = [Buf(f"qkst{i}") for i in range(4)]
        v_st = [A.alloc(D) for _ in range(2)]
        VST = [Buf(f"vst{i}") for i in range(2)]
        fl3 = fl.rearrange("p (b h) -> p b h", h=8)

        def load_x(c):
            s = c % 2
            for b in range(4):
                r0 = c * 512 + b * 128
                P.dma("sp", f"xin{s}_{b}", xin[s][:, b, :], x_d[r0:r0 + 128, :], writes=[XIN[s][b]])

        load_x(0)
        ev = [0]
        mmb = [0]
        for c in range(NCH):
            s = c % 2
            if c + 1 < NCH:
                load_x(c + 1)
            for b in range(4):
                gb = c * 4 + b
                k = gb % 2
                rs, rsb = rms_scale(xin[s][:, b, :], [XIN[s][b]], D)
                P.op("dve", lambda e, s=s, b=b, k=k, rs=rs: e.scalar_tensor_tensor(
                    out=ub[k], in0=xin[s][:, b, :], scalar=rs, in1=g_mix, op0=ALU.mult, op1=ALU.mult),
                    reads=[XIN[s][b], rsb, CONST, GC], writes=[UB[k]])
                transposes_bf(ub[k], [UB[k]], gb % 2, uT[s][:, :, b * 128:(b + 1) * 128], [UT[s][b]],
                              copy_eng=("act" if gb % 2 == 0 else "dve"))
            for n in range(16):
                if n < 4:
                    col, sc = n * 128, 0.125
                elif n < 8:
                    col, sc = 512 + (n - 4) * 128, 1.0
                elif n < 12:
                    col, sc = 1536 + (n - 8) * 128, 0.125
                else:
                    col, sc = 2048 + (n - 12) * 128, 1.0
                bk = 2 + mmb[0] % 4
                mmb[0] += 1

                def f(e, s=s, col=col, bk=bk):
                    for fc in range(8):
                        ins = e.matmul(bank(bk), lhsT=w_in_sb[:, fc, col:col + 128], rhs=uT[s][:, fc, :],
                                       start=(fc == 0), stop=(fc == 7))
                    return ins
                P.op("pe", f, reads=UT[s] + [WINSB], writes=[bankB[bk]])
                qs = ev[0] % 4
                ev[0] += 1
                if ev[0] % 2 == 0:
                    P.op("act", lambda e, qs=qs, bk=bk, sc=sc: e.activation(out=qk_st[qs], in_=bank(bk), func=AF.Copy, scale=sc),
                         reads=[bankB[bk]], writes=[QKST[qs]])
                else:
                    P.op("dve", lambda e, qs=qs, bk=bk, sc=sc: e.tensor_scalar(out=qk_st[qs], in0=bank(bk), scalar1=sc, scalar2=0.0, op0=ALU.mult, op1=ALU.add),
                         reads=[bankB[bk]], writes=[QKST[qs]])
                P.dma("sp", f"qkst{qs}", qkT_d[n * 128:(n + 1) * 128, c * 512:(c + 1) * 512], qk_st[qs],
                      reads=[QKST[qs]])
            for b in range(4):
                gb = c * 4 + b
                vs_ = gb % 2
                for half, col in ((0, 1024), (1, 2560)):
                    bk = 2 + mmb[0] % 4
                    mmb[0] += 1

                    def f(e, s=s, b=b, col=col, bk=bk):
                        for fc in range(8):
                            ins = e.matmul(bank(bk), lhsT=uT[s][:, fc, b * 128:(b + 1) * 128],
                                           rhs=w_in_sb[:, fc, col:col + 512], start=(fc == 0), stop=(fc == 7))
                        return ins
                    P.op("pe", f, reads=[UT[s][b], WINSB], writes=[bankB[bk]])
                    if half == 0:
                        P.op("act", lambda e, vs_=vs_, bk=bk: e.copy(out=v_st[vs_][:, 0:512], in_=bank(bk)),
                             reads=[bankB[bk]], writes=[VST[vs_]])
                    else:
                        P.op("dve", lambda e, vs_=vs_, bk=bk: e.tensor_copy(out=v_st[vs_][:, 512:1024], in_=bank(bk)),
                             reads=[bankB[bk]], writes=[VST[vs_]])
                bk = 2 + mmb[0] % 4
                mmb[0] += 1

                def f(e, s=s, b=b, bk=bk):
                    for fc in range(8):
                        ins = e.matmul(bank(bk)[:, 0:8], lhsT=uT[s][:, fc, b * 128:(b + 1) * 128],
                                       rhs=w_in_sb[:, fc, 3072:3080], start=(fc == 0), stop=(fc == 7))
                    return ins
                P.op("pe", f, reads=[UT[s][b], WINSB], writes=[bankB[bk]])
                P.op("dve", lambda e, gb=gb, bk=bk: e.tensor_tensor(out=fl3[:, gb, :], in0=bank(bk)[:, 0:8], in1=bfor, op=ALU.add),
                     reads=[bankB[bk], CONST, GC], writes=[FL])
                r0 = gb * 128
                P.dma("sp", f"vst{vs_}", vs_d[r0:r0 + 128, :], v_st[vs_], reads=[VST[vs_]])

        NF = NB * 8
        P.op("act", lambda e: e.activation(out=fl, in_=fl, func=AF.Exp, scale=-1.0), reads=[FL], writes=[FL])
        P.op("act", lambda e: e.activation(out=fl, in_=fl, func=AF.Ln, bias=1.0), reads=[FL], writes=[FL])
        P.op("pe", lambda e: e.matmul(bank(0)[:, 0:NF], lhsT=tri_f, rhs=fl, start=True, stop=True),
             reads=[FL, CONST], writes=[bankB[0]])
        P.op("pe", lambda e: e.matmul(bank(1)[:, 0:NF], lhsT=ones_f, rhs=fl, start=True, stop=True),
             reads=[FL, CONST], writes=[bankB[1]])
        off3 = off.rearrange("p (b h) -> p b h", h=8)
        af3 = a_full.rearrange("p (b h) -> p b h", h=8)
        tot3 = bank(1)[:, 0:NF].rearrange("p (b h) -> p b h", h=8)
        P.op("dve", lambda e: e.memset(off3[:, 0, :], 0.0), writes=[OFFB])
        for b in range(1, NB):
            P.op("dve", lambda e, b=b: e.tensor_tensor(out=off3[:, b, :], in0=tot3[:, b - 1, :], in1=off3[:, b - 1, :], op=ALU.add),
                 reads=[bankB[1], OFFB], writes=[OFFB])
        P.op("dve", lambda e: e.tensor_tensor(out=a_full, in0=bank(0)[:, 0:NF], in1=off, op=ALU.add),
             reads=[bankB[0], OFFB], writes=[AFULL])

        P.barrier()
        A.release(base_mark)

        VW = 72
        kTp = [A.alloc(S) for _ in range(2)]
        qp = [[A.alloc(S) for _ in range(2)] for _ in range(2)]
        vv = [A.alloc(NB * 128).rearrange("p (b w) -> p b w", w=128) for _ in range(2)]
        HBV = [Buf(f"headv{i}") for i in range(2)]
        HB = [Buf(f"head{i}") for i in range(2)]
        pT = [A.alloc(512) for _ in range(3)]
        PT = [Buf(f"pT{i}") for i in range(3)]
        e_t = [A.alloc(512, F32) for _ in range(2)]
        ET = [Buf(f"et{i}") for i in range(2)]
        sp_t = [A.alloc(512) for _ in range(3)]
        SPT = [Buf(f"spt{i}") for i in range(3)]
        cum = A.alloc(512)
        CUM = Buf("cum")
        ot_sb = [A.alloc(512, F32) for _ in range(2)]
        OTSB = [Buf(f"otsb{i}") for i in range(2)]
        o_tok = [A.alloc(4 * 64, F32).rearrange("p (b d) -> p b d", d=64) for _ in range(2)]
        OTOK = [Buf(f"otok{i}") for i in range(2)]
        rec = [A.alloc(4, F32) for _ in range(2)]
        REC = [Buf(f"rec{i}") for i in range(2)]
        sq_t = [A.alloc(512) for _ in range(2)]
        SQT = [Buf(f"sqt{i}") for i in range(2)]
        nmx = A.alloc(4 * 16, F32).rearrange("p (a c) -> p a c", c=16)
        nm4 = A.alloc(4, F32)
        thr = A.alloc(2, F32)
        NRM = Buf("nrm")
        THR = Buf("thr")
        minc = A.alloc(1, F32)
        MINC = Buf("minc")
        failf = A.alloc(1, F32)
        tmpf = A.alloc(1, F32)
        FAILB = Buf("failf")
        I32 = mybir.dt.int32
        flags = [A.alloc(NCH, F32).bitcast(I32) for _ in range(2)]
        FLG = [Buf(f"flg{i}") for i in range(2)]
        biasj = [A.alloc(NB, F32) for _ in range(2)]
        BIASJ = [Buf(f"biasj{i}") for i in range(2)]
        FOX_WS = [w for w in (3, 5, 7) if w < NCH - 1]
        NV = len(FOX_WS) + 1
        flagsF = [A.alloc(8, F32).bitcast(I32) for _ in range(2)]
        FLGF = [Buf(f"flgF{i}") for i in range(2)]
        offc = A.alloc(NCH, F32)
        dmat = A.alloc(NCH, F32)
        okf = A.alloc(8, F32)
        fvf = A.alloc(8, F32)
        OFFC = Buf("offc")
        off4 = off.rearrange("p (c f h) -> p c f h", f=4, h=8)

        for i in range(2):
            def f(e, i=i):
                e.memset(vv[i][:, :, 64:65], 1.0)
                e.memset(vv[i][:, :, 65:128], 0.0)
                e.memset(qp[i][0][64:128, :], 0.0)
                return e.memset(qp[i][1][0:64, :], 0.0)
            P.op("pool", f, writes=[HB[i], HBV[i]])
        late_conversions()

        pairs = [("A", p) for p in range(4)] + [("B", p) for p in range(4)]

        def load_pair(pi):
            typ, p = pairs[pi]
            s = pi % 2
            qrow = (0 if typ == "A" else 1024) + p * 128
            krow = (512 if typ == "A" else 1536) + p * 128
            P.dma("sp", f"hk{s}", kTp[s], qkT_d[krow:krow + 128, :], writes=[HB[s]])
            P.dma("sp", f"hq0{s}", qp[s][0][0:64, :], qkT_d[qrow:qrow + 64, :], writes=[HB[s]])
            P.dma("sp", f"hq1{s}", qp[s][1][64:128, :], qkT_d[qrow + 64:qrow + 128, :], writes=[HB[s]])

        def load_v(pi, g):
            typ, p = pairs[pi]
            vcol = (0 if typ == "A" else 512) + p * 128
            P.dma("sp", f"hv{g}", vv[g][:, :, 0:64],
                  vs_d[:, vcol + g * 64:vcol + (g + 1) * 64].rearrange("(b p) d -> p b d", p=128), writes=[HBV[g]])

        S_BK = [0, 1, 2]
        OT_BK = [4, 5]
        TR_BK = 6
        tr3 = bank(TR_BK).rearrange("p (b c) -> p b c", c=128)

        def evac_o(typ, h, j, slot_h, queue="sp", extra=()):
            ob = OT_BK[j % 2]
            sl = j % 2
            nrow = 65
            P.op("act", lambda e: e.copy(out=ot_sb[sl][0:nrow, :], in_=bank(ob)[0:nrow, :]),
                 reads=[bankB[ob]], writes=[OTSB[sl]])

            def f(e):
                for b in range(4):
                    ins = e.transpose(out=tr3[:, b, 0:nrow], in_=ot_sb[sl][0:nrow, b * 128:(b + 1) * 128],
                                      identity=ident_f[0:nrow, 0:nrow])
                return ins
            P.op("pe", f, reads=[OTSB[sl], CONST], writes=[bankB[TR_BK]])
            if typ == "A":
                P.op("dve", lambda e: e.reciprocal(out=rec[sl].rearrange("p (b o) -> p b o", o=1), in_=tr3[:, :, 64:65]),
                     reads=[bankB[TR_BK]], writes=[REC[sl]])
                P.op("dve", lambda e: e.tensor_tensor(out=o_tok[sl], in0=tr3[:, :, 0:64],
                                                      in1=rec[sl].rearrange("p (b o) -> p b o", o=1).to_broadcast([128, 4, 64]),
                                                      op=ALU.mult),
                     reads=[bankB[TR_BK], REC[sl]], writes=[OTOK[sl]])
                col = h * 64
            else:
                P.op("dve", lambda e: e.tensor_copy(out=o_tok[sl], in_=tr3[:, :, 0:64]),
                     reads=[bankB[TR_BK]], writes=[OTOK[sl]])
                col = 512 + h * 64
            return P.dma(queue, f"otok{sl}", os_d[j * 512:(j + 1) * 512, col:col + 64].rearrange("(b p) d -> p b d", p=128),
                         o_tok[sl], reads=[OTOK[sl]], extra=extra)

        fox_ctr = [0]

        def fox_head(pi, g):
            typ, p = pairs[pi]
            h = p * 2 + g
            s = pi % 2
            q_, k_, v_ = qp[s][g], kTp[s], vv[g]
            fpar = (pi * 2 + g) % 2
            P.op("dve", lambda e: e.tensor_copy(out=offc, in_=off4[:, :, 0, h]), reads=[OFFB], writes=[OFFC])
            for vi, w in enumerate(FOX_WS):
                n_ = NCH - 1 - w
                P.op("dve", lambda e, w=w, n_=n_: e.tensor_tensor(out=dmat[:, 0:n_], in0=offc[:, w + 1:NCH], in1=offc[:, 1:NCH - w], op=ALU.subtract),
                     reads=[OFFC], writes=[OFFC])
                P.op("dve", lambda e, vi=vi, n_=n_: e.tensor_reduce(out=okf[:, vi:vi + 1], in_=dmat[:, 0:n_], axis=AX.X, op=ALU.min),
                     reads=[OFFC], writes=[OFFC])
            if FOX_WS:
                P.op("dve", lambda e: e.tensor_scalar(out=okf[:, 0:NV - 1], in0=okf[:, 0:NV - 1], scalar1=thr[:, g:g + 1], scalar2=0.0,
                                                      op0=ALU.is_gt, op1=ALU.add), reads=[OFFC, THR], writes=[OFFC])
            P.op("dve", lambda e: e.memset(fvf[:, NV:NV + 1], 1.0), writes=[OFFC])
            for vi in range(NV - 1):
                P.op("dve", lambda e, vi=vi: e.tensor_tensor(out=fvf[:, vi:vi + 1], in0=okf[:, vi:vi + 1], in1=fvf[:, NV:NV + 1], op=ALU.mult),
                     reads=[OFFC], writes=[OFFC])
                P.op("dve", lambda e, vi=vi: e.tensor_tensor(out=fvf[:, NV:NV + 1], in0=fvf[:, NV:NV + 1], in1=fvf[:, vi:vi + 1], op=ALU.subtract),
                     reads=[OFFC], writes=[OFFC])
            P.op("dve", lambda e: e.tensor_copy(out=fvf[:, NV - 1:NV], in_=fvf[:, NV:NV + 1]), reads=[OFFC], writes=[OFFC])
            P.op("dve", lambda e: e.tensor_copy(out=flagsF[fpar][:, 0:NV], in_=fvf[:, 0:NV]), reads=[OFFC], writes=[FLGF[fpar]])

            def variant(w):
                tiles = []
                for j in range(NCH):
                    b0 = 0 if w is None else 4 * max(0, j - w)
                    for b in range(b0, 4 * j):
                        tiles.append((j, b, None, b == b0))
                    for i in range(4):
                        tiles.append((j, 4 * j + i, i, (4 * j == b0 and i == 0)))
                T = len(tiles)
                base = fox_ctr[0]
                fox_ctr[0] += T

                def stA(t):
                    j, b, i, first = tiles[t]
                    if first:
                        nkb = 4 * j + 4
                        bs = j % 2
                        P.op("dve", lambda e: e.tensor_scalar(out=biasj[bs][:, 0:nkb], in0=af3[:, 0:nkb, h],
                                                              scalar1=off3[:, 4 * j + 2, h:h + 1], scalar2=0.0, op0=ALU.subtract, op1=ALU.add),
                             reads=[AFULL, OFFB], writes=[BIASJ[bs]])
                    bk = S_BK[(base + t) % 3]
                    c0 = 0 if i is None else 128 * i

                    def f(e):
                        ins = e.matmul(bank(bk)[:, c0:512], lhsT=k_[:, b * 128:(b + 1) * 128],
                                       rhs=q_[:, j * 512 + c0:(j + 1) * 512], start=True, stop=(i is None))
                        if i is not None:
                            ins = e.matmul(bank(bk)[:, c0:c0 + 128], lhsT=ident_bf, rhs=mask_fox, start=False, stop=True)
                        return ins
                    P.op("pe", f, reads=[HB[s], CONST], writes=[bankB[bk]])

                def stB(t):
                    j, b, i, first = tiles[t]
                    bk = S_BK[(base + t) % 3]
                    c0 = 0 if i is None else 128 * i
                    ps = (base + t) % 3
                    P.op("act", lambda e: e.activation(out=pT[ps][:, c0:512], in_=bank(bk)[:, c0:512], func=AF.Exp,
                                                       bias=biasj[j % 2][:, b:b + 1], scale=1.0),
                         reads=[bankB[bk], BIASJ[j % 2]], writes=[PT[ps]])

                def stC(t):
                    j, b, i, first = tiles[t]
                    c0 = 0 if i is None else 128 * i
                    ps = (base + t) % 3
                    ob = OT_BK[j % 2]
                    last = (i == 3)
                    P.op("pe", lambda e: e.matmul(bank(ob)[:, c0:512], lhsT=v_[:, b, :], rhs=pT[ps][:, c0:512],
                                                  start=first, stop=last),
                         reads=[PT[ps], HBV[g]], writes=[bankB[ob]])
                    if last:
                        evac_o(typ, h, j, s, queue="pool")

                for t in range(T + 1):
                    if t < T:
                        stA(t)
                        stB(t)
                    if t >= 1:
                        stC(t - 1)

            for vi, w in enumerate(FOX_WS + [None]):
                P.region_begin(("pe", "act", "dve", "pool"), flagsF[fpar][0:1, vi:vi + 1], [FLGF[fpar]])
                variant(w)
                P.region_end()

        def sb_bounds(pi, mult=1.05):
            s = pi % 2
            cnt_ = [0]

            def one(src, lhs_list, rows):
                for c in range(NCH):
                    k = cnt_[0] % 2
                    cnt_[0] += 1
                    P.op("act", lambda e, c=c, k=k: e.activation(out=sq_t[k], in_=src[:, c * 512:(c + 1) * 512], func=AF.Square),
                         reads=[HB[s]], writes=[SQT[k]])
                    for lhs, row in zip(lhs_list, rows):
                        bk = cnt_[0] % 4
                        cnt_[0] += 1
                        P.op("pe", lambda e, lhs=lhs, k=k, bk=bk: e.matmul(bank(bk), lhsT=lhs, rhs=sq_t[k], start=True, stop=True),
                             reads=[SQT[k], CONST], writes=[bankB[bk]])
                        P.op("dve", lambda e, row=row, c=c, bk=bk: e.tensor_reduce(out=nmx[:, row, c:c + 1], in_=bank(bk), axis=AX.X, op=ALU.max),
                             reads=[bankB[bk]], writes=[NRM])
            one(kTp[s], [sel0, sel1], [0, 1])
            one(qp[s][0], [ones_bf], [2])
            one(qp[s][1], [ones_bf], [3])
            P.op("dve", lambda e: e.tensor_reduce(out=nm4, in_=nmx[:, :, 0:NCH], axis=AX.X, op=ALU.max), reads=[NRM], writes=[NRM])
            P.op("dve", lambda e: e.tensor_tensor(out=nm4[:, 0:2], in0=nm4[:, 0:2], in1=nm4[:, 2:4], op=ALU.mult), reads=[NRM], writes=[NRM])
            P.op("act", lambda e: e.activation(out=nm4[:, 0:2], in_=nm4[:, 0:2], func=AF.Ln), reads=[NRM], writes=[NRM])
            P.op("act", lambda e: e.activation(out=nm4[:, 0:2], in_=nm4[:, 0:2], func=AF.Exp, scale=0.5), reads=[NRM], writes=[NRM])
            P.op("dve", lambda e: e.tensor_scalar(out=thr, in0=nm4[:, 0:2], scalar1=mult, scalar2=108.0, op0=ALU.mult, op1=ALU.add),
                 reads=[NRM], writes=[THR])

        sb_ctr = [0]

        def sb_head(pi, g):
            typ, p = pairs[pi]
            h = p * 2 + g
            s = pi % 2
            q_, k_, v_ = qp[s][g], kTp[s], vv[g]
            ZB = [0, 1, 2, 3]
            fpar = (pi * 2 + g) % 2

            def stA(tl, t):
                j, b, i, first = tl
                bk = ZB[t % 4]
                c0 = 0 if i is None else 128 * i
                P.op("pe", lambda e: e.matmul(bank(bk)[:, c0:512], lhsT=k_[:, b * 128:(b + 1) * 128],
                                              rhs=q_[:, j * 512 + c0:(j + 1) * 512], start=True, stop=False),
                     reads=[HB[s]], writes=[bankB[bk]])
                es_ = t % 2
                P.op("act", lambda e: e.activation(out=e_t[es_][:, c0:512], in_=bank(bk)[:, c0:512], func=AF.Exp),
                     reads=[bankB[bk]], writes=[ET[es_]])
                ss_ = t % 3
                P.op("act", lambda e: e.activation(out=sp_t[ss_][:, c0:512], in_=e_t[es_][:, c0:512], func=AF.Ln, bias=1.0),
                     reads=[ET[es_]], writes=[SPT[ss_]])
                if i is not None:
                    P.op("pool", lambda e: e.tensor_tensor(out=sp_t[ss_][:, c0:c0 + 128], in0=sp_t[ss_][:, c0:c0 + 128],
                                                           in1=m01_sb, op=ALU.mult),
                         reads=[SPT[ss_], CONST], writes=[SPT[ss_]])

            def stB(tl, t):
                j, b, i, first = tl
                bk = ZB[t % 4]
                c0 = 0 if i is None else 128 * i
                ss_ = t % 3

                def f(e):
                    ins = e.matmul(bank(bk)[:, c0:512], lhsT=negtri, rhs=sp_t[ss_][:, c0:512], start=False,
                                   stop=False)
                    if not first:
                        ins = e.matmul(bank(bk)[:, c0:512], lhsT=negones, rhs=cum[:, c0:512], start=False, stop=(i is None))
                    if i is not None:
                        ins = e.matmul(bank(bk)[:, c0:c0 + 128], lhsT=ident_bf, rhs=mask_sb, start=False, stop=True)
                    return ins
                P.op("pe", f, reads=[SPT[ss_], CUM, CONST], writes=[bankB[bk]])
                if first:
                    def g_(e):
                        e.memset(cum[:, 0:c0], 0.0)
                        return e.tensor_copy(out=cum[:, c0:512], in_=sp_t[ss_][:, c0:512])
                    P.op("pool", g_, reads=[SPT[ss_]], writes=[CUM])
                else:
                    P.op("pool", lambda e: e.tensor_tensor(out=cum[:, c0:512], in0=cum[:, c0:512], in1=sp_t[ss_][:, c0:512], op=ALU.add),
                         reads=[SPT[ss_], CUM], writes=[CUM])
                P.op("act", lambda e: e.activation(out=pT[ss_][:, c0:512], in_=bank(bk)[:, c0:512], func=AF.Exp),
                     reads=[bankB[bk]], writes=[PT[ss_]])

            def stC(tl, t):
                j, b, i, first = tl
                c0 = 0 if i is None else 128 * i
                ss_ = t % 3
                ob = OT_BK[j % 2]

                def f(e):
                    if first:
                        e.matmul(bank(ob), lhsT=zeros_bf[:, 0:128], rhs=zeros_bf, start=True, stop=False)
                    return e.matmul(bank(ob)[:, c0:512], lhsT=v_[:, b, :], rhs=pT[ss_][:, c0:512],
                                    start=False, stop=False)
                P.op("pe", f, reads=[PT[ss_], HBV[g], CONST], writes=[bankB[ob]])

            def flag_ops(j):
                P.op("pe", lambda e: e.matmul(bank(7), lhsT=ones_bf, rhs=cum, start=True, stop=True),
                     reads=[CUM, CONST], writes=[bankB[7]])
                P.op("dve", lambda e: e.tensor_reduce(out=minc, in_=bank(7), axis=AX.X, op=ALU.min),
                     reads=[bankB[7]], writes=[MINC])
                P.op("dve", lambda e: e.tensor_tensor(out=flags[fpar][:, j:j + 1], in0=thr[:, g:g + 1], in1=minc, op=ALU.is_gt),
                     reads=[MINC, THR], writes=[FLG[fpar]])

            def pipeline(tls, flag_after=None, j=None):
                T = len(tls)
                base = sb_ctr[0]
                sb_ctr[0] += T
                for t in range(T + 2):
                    if t < T:
                        stA(tls[t], base + t)
                    if 1 <= t <= T:
                        stB(tls[t - 1], base + t - 1)
                        if flag_after is not None and t - 1 == flag_after:
                            flag_ops(j)
                    if t >= 2:
                        stC(tls[t - 2], base + t - 2)

            NU = 3

            def chunk_tiles(j):
                ut = [(j, 4 * j + i, i, i == 3) for i in (3, 2, 1, 0)]
                ut += [(j, b, None, False) for b in range(4 * j - 1, max(4 * j - 1 - NU, -1), -1)]
                rt = [(j, b, None, False) for b in range(4 * j - 1 - NU, -1, -1)]
                return ut, rt

            def close_bank(j):
                ob = OT_BK[j % 2]
                P.op("pe", lambda e, ob=ob: e.matmul(bank(ob), lhsT=zeros_bf[:, 0:128], rhs=zeros_bf, start=False, stop=True),
                     reads=[CONST], writes=[bankB[ob]])

            P.op("dve", lambda e: e.memset(failf, 1.0 if FORCE_SB_FULL else 0.0), writes=[FAILB])
            wtoks = {}
            for j in range(NCH):
                ut, rt = chunk_tiles(j)
                pipeline(ut)
                if rt:
                    P.op("pe", lambda e: e.matmul(bank(7), lhsT=ones_bf, rhs=cum, start=True, stop=True),
                         reads=[CUM, CONST], writes=[bankB[7]])
                    P.op("dve", lambda e: e.tensor_reduce(out=minc, in_=bank(7), axis=AX.X, op=ALU.min),
                         reads=[bankB[7]], writes=[MINC])
                    P.op("dve", lambda e: e.tensor_tensor(out=tmpf, in0=thr[:, g:g + 1], in1=minc, op=ALU.is_gt),
                         reads=[MINC, THR], writes=[FAILB])
                    P.op("dve", lambda e: e.tensor_tensor(out=failf, in0=failf, in1=tmpf, op=ALU.max),
                         reads=[FAILB], writes=[FAILB])
                close_bank(j)
                wtoks[j] = evac_o(typ, h, j, s)
            P.op("dve", lambda e: e.tensor_copy(out=flags[fpar][:, 0:1], in_=failf), reads=[FAILB], writes=[FLG[fpar]])
            P.region_begin(("pe", "act", "pool", "dve"), flags[fpar][0:1, 0:1], [FLG[fpar]])
            for j in range(NCH):
                ut, rt = chunk_tiles(j)
                if not rt:
                    continue
                pipeline(ut + rt)
                close_bank(j)
                evac_o(typ, h, j, s, queue="pool", extra=[wtoks[j]])
            P.region_end()

        load_pair(0)
        load_v(0, 0)
        load_v(0, 1)
        for pi in range(8):
            if pi + 1 < 8:
                load_pair(pi + 1)
            for g in range(2):
                if pairs[pi][0] == "A":
                    if g == 0:
                        sb_bounds(pi, 2.1)
                    fox_head(pi, g)
                else:
                    if g == 0:
                        sb_bounds(pi)
                    sb_head(pi, g)
                if pi + 1 < 8:
                    load_v(pi + 1, g)

        P.barrier()
        A.release(mark0)

        def wload(name, src_b, wbuf, ncols):
            t = A.alloc(8 * ncols).rearrange("p (c n) -> p c n", n=ncols)
            B_ = Buf(name)
            P.dma("sp", name, t, src_b.rearrange("(c p) n -> p c n", p=128), reads=[wbuf], writes=[B_])
            return t, B_

        w_out_sb, WOUT = wload("w_out_sb", w_out_b, WB["out"], D)
        w_xq_sb, WXQ = wload("w_xq_sb", w_xq_b, WB["xq"], D)
        w_xo_sb, WXO = wload("w_xo_sb", w_xo_b, WB["xo"], D)
        wr_sb = A.alloc(8 * 20, F32).rearrange("p (c n) -> p c n", n=20)
        WR = Buf("wr")
        P.dma("sp", "wr_g", wr_sb[:, :, 0:4], w_rg_d.rearrange("(c p) n -> p c n", p=128), writes=[WR])
        P.dma("sp", "wr_e", wr_sb[:, :, 4:20], w_re_d.rearrange("(c p) n -> p c n", p=128), writes=[WR])
        kxT = A.alloc(8 * NMEM).rearrange("p (c m) -> p c m", m=NMEM)
        vx = A.alloc(2 * D).rearrange("p (b n) -> p b n", n=D)
        KV = Buf("kv")

        p3_mark = A.mark()
        w_xkv_sb, WXKV = wload("w_xkv_sb", w_xkv_b, WB["xkv"], 2 * D)
        mem_in = A.alloc(2 * D, F32).rearrange("p (b d) -> p b d", d=D)
        MEMIN = Buf("memin")
        P.dma("sp", "memin", mem_in, mem_d.rearrange("(b p) d -> p b d", p=128), writes=[MEMIN])
        mn_b = A.alloc(D)
        MNB = Buf("mnb")
        mT = A.alloc(8 * NMEM).rearrange("p (c m) -> p c m", m=NMEM)
        MT = Buf("mT")
        for b in range(2):
            rs, rsb = rms_scale(mem_in[:, b, :], [MEMIN], D)
            P.op("dve", lambda e, b=b, rs=rs: e.scalar_tensor_tensor(out=mn_b, in0=mem_in[:, b, :], scalar=rs, in1=g_mem,
                                                                     op0=ALU.mult, op1=ALU.mult),
                 reads=[MEMIN, rsb, CONST, GC], writes=[MNB])
            transposes_bf(mn_b, [MNB], 0, mT[:, :, b * 128:(b + 1) * 128], [MT])
        for n in range(8):
            bk = 2 + n % 4

            def f(e, n=n, bk=bk):
                for fc in range(8):
                    ins = e.matmul(bank(bk)[:, 0:NMEM], lhsT=w_xkv_sb[:, fc, n * 128:(n + 1) * 128], rhs=mT[:, fc, :],
                                   start=(fc == 0), stop=(fc == 7))
                return ins
            P.op("pe", f, reads=[MT, WXKV], writes=[bankB[bk]])
            P.op("act", lambda e, n=n, bk=bk: e.copy(out=kxT[:, n, :], in_=bank(bk)[:, 0:NMEM]), reads=[bankB[bk]], writes=[KV])
        for b in range(2):
            for half in range(2):
                bk = 2 + (b * 2 + half) % 4

                def f(e, b=b, half=half, bk=bk):
                    for fc in range(8):
                        ins = e.matmul(bank(bk), lhsT=mT[:, fc, b * 128:(b + 1) * 128],
                                       rhs=w_xkv_sb[:, fc, D + half * 512:D + (half + 1) * 512], start=(fc == 0), stop=(fc == 7))
                    return ins
                P.op("pe", f, reads=[MT, WXKV], writes=[bankB[bk]])
                P.op("dve", lambda e, b=b, half=half, bk=bk: e.tensor_copy(out=vx[:, b, half * 512:(half + 1) * 512], in_=bank(bk)),
                     reads=[bankB[bk]], writes=[KV])
        P.barrier()
        A.release(p3_mark)

        G = 8
        hbufs = [A.alloc(G * ROWW, F32).rearrange("p (b d) -> p b d", d=ROWW) for _ in range(2)]
        HBUFS = [[Buf(f"h{i}_{b}") for b in range(G)] for i in range(2)]
        HBUF2S = [[Buf(f"h2_{i}_{b}") for b in range(G)] for i in range(2)]
        o_in = [A.alloc(D, F32) for _ in range(2)]
        OIN = [Buf(f"oin{i}") for i in range(2)]
        nb_ = [A.alloc(D) for _ in range(2)]
        NBB = [Buf(f"nb{i}") for i in range(2)]
        n32, N32 = o_in[0], OIN[0]
        tT = A.alloc(8 * 512).rearrange("p (c t) -> p c t", t=512)
        TT = [Buf(f"tT{b}") for b in range(4)]
        qxT = A.alloc(8 * 512).rearrange("p (c t) -> p c t", t=512)
        QXT = Buf("qxT")
        oxT = tT
        pxT = [A.alloc(2 * 512).rearrange("p (m t) -> p m t", t=512) for _ in range(2)]
        PXT = [Buf(f"pxT{i}") for i in range(2)]
        rden = [A.alloc(512, F32) for _ in range(2)]
        RDEN = [Buf(f"rden{i}") for i in range(2)]
        h32T, H32T = o_in[1].rearrange("p (c t) -> p c t", t=128), OIN[1]
        lg = A.alloc(G * 20, F32).rearrange("p (g n) -> p g n", n=20)
        gate = A.alloc(G * 16, F32).rearrange("p (g n) -> p g n", n=16)
        RT = Buf("router")
        GATE = Buf("gate")
        r_mg = A.alloc(G, F32)
        r_sh = A.alloc(G * 4, F32).rearrange("p (g n) -> p g n", n=4)
        r_gv = A.alloc(G, F32)
        r_oh = A.alloc(G * 4, F32).rearrange("p (g n) -> p g n", n=4)
        r_ohk = A.alloc(G * 4, F32).rearrange("p (g n) -> p g n", n=4)
        r_m0 = A.alloc(G, F32)
        oh_bf = A.alloc(G * 4)
        basec = A.alloc(G * 4, F32).rearrange("p (g n) -> p g n", n=4)
        dtmp = A.alloc(G * 4, F32).rearrange("p (g n) -> p g n", n=4)
        dest_f = A.alloc(G, F32)
        dest_i = [A.alloc(G, F32).bitcast(I32) for _ in range(2)]
        DEST = [Buf(f"dest{i}") for i in range(2)]
        r_ml = A.alloc(G * 16, F32).rearrange("p (g n) -> p g n", n=16)
        r_ml2 = A.alloc(G * 16, F32).rearrange("p (g n) -> p g n", n=16)
        r_m1 = A.alloc(G, F32)
        r_m2 = A.alloc(G, F32)
        r_oh1 = A.alloc(G * 16, F32).rearrange("p (g n) -> p g n", n=16)
        r_oh2 = A.alloc(G * 16, F32).rearrange("p (g n) -> p g n", n=16)
        r_e = A.alloc(G, F32)
        r_w1 = A.alloc(G, F32)
        r_w2 = A.alloc(G, F32)
        CNT = Buf("cnt")

        NSC = S // (G * 128)
        mmc = [0]

        def mmbank():
            b = 2 + mmc[0] % 4
            mmc[0] += 1
            return b

        P.op("dve", lambda e: e.tensor_copy(out=cnt_run, in_=goff), reads=[CONST2], writes=[CNT])

        for sc in range(NSC):
            tok0 = sc * G * 128
            hbuf, HBUF, HBUF2 = hbufs[sc % 2], HBUFS[sc % 2], HBUF2S[sc % 2]
            for b in range(G):
                r0 = tok0 + b * 128
                P.dma("sp", f"hx{sc % 2}_{b}", hbuf[:, b, 0:D], x_d[r0:r0 + 128, :], writes=[HBUF[b], HBUF2[b]])
            for half in range(2):
                for b4 in range(4):
                    b = half * 4 + b4
                    r0 = tok0 + b * 128
                    k = b % 2
                    P.dma("sp", f"oin{k}", o_in[k], os_d[r0:r0 + 128, :], writes=[OIN[k]])
                    rsa, rsab = rms_scale(o_in[k][:, 0:512], [OIN[k]], 512)
                    P.op("dve", lambda e, k=k, rsa=rsa: e.scalar_tensor_tensor(out=nb_[k][:, 0:512], in0=o_in[k][:, 0:512], scalar=rsa,
                                                                               in1=g_cat[:, 0:512], op0=ALU.mult, op1=ALU.mult),
                         reads=[OIN[k], rsab, CONST, GC], writes=[NBB[k]])
                    rsb_, rsbb = rms_scale(o_in[k][:, 512:1024], [OIN[k]], 512)
                    P.op("dve", lambda e, k=k, rsb_=rsb_: e.scalar_tensor_tensor(out=nb_[k][:, 512:1024], in0=o_in[k][:, 512:1024], scalar=rsb_,
                                                                                 in1=g_cat[:, 512:1024], op0=ALU.mult, op1=ALU.mult),
                         reads=[OIN[k], rsbb, CONST, GC], writes=[NBB[k]])
                    transposes_bf(nb_[k], [NBB[k]], b % 2, tT[:, :, b4 * 128:(b4 + 1) * 128], [TT[b4]],
                                  copy_eng=("act" if b % 2 == 0 else "dve"))
                for b4 in range(4):
                    b = half * 4 + b4
                    for nh in range(2):
                        bk = mmbank()

                        def f(e, b4=b4, nh=nh, bk=bk):
                            for fc in range(8):
                                ins = e.matmul(bank(bk), lhsT=tT[:, fc, b4 * 128:(b4 + 1) * 128],
                                               rhs=w_out_sb[:, fc, nh * 512:(nh + 1) * 512], start=(fc == 0), stop=(fc == 7))
                            return ins
                        P.op("pe", f, reads=[TT[b4], WOUT], writes=[bankB[bk]])
                        P.op("dve", lambda e, b=b, nh=nh, bk=bk, hbuf=hbuf: e.tensor_tensor(out=hbuf[:, b, nh * 512:(nh + 1) * 512],
                                                                                 in0=hbuf[:, b, nh * 512:(nh + 1) * 512], in1=bank(bk), op=ALU.add),
                             reads=[bankB[bk], HBUF[b], HBUF2[b]], writes=[HBUF[b], HBUF2[b]])
                for b4 in range(4):
                    b = half * 4 + b4
                    k = b % 2
                    rs, rsb = rms_scale(hbuf[:, b, 0:D], [HBUF[b], HBUF2[b]], D)
                    P.op("dve", lambda e, b=b, k=k, rs=rs, hbuf=hbuf: e.scalar_tensor_tensor(out=nb_[k], in0=hbuf[:, b, 0:D], scalar=rs, in1=g_x,
                                                                                  op0=ALU.mult, op1=ALU.mult),
                         reads=[HBUF[b], HBUF2[b], rsb, CONST, GC], writes=[NBB[k]])
                    transposes_bf(nb_[k], [NBB[k]], b % 2, tT[:, :, b4 * 128:(b4 + 1) * 128], [TT[b4]],
                                  copy_eng=("act" if b % 2 == 0 else "dve"))
                for n in range(8):
                    bk = mmbank()

                    def f(e, n=n, bk=bk):
                        for fc in range(8):
                            ins = e.matmul(bank(bk), lhsT=w_xq_sb[:, fc, n * 128:(n + 1) * 128], rhs=tT[:, fc, :],
                                           start=(fc == 0), stop=(fc == 7))
                        return ins
                    P.op("pe", f, reads=TT + [WXQ], writes=[bankB[bk]])
                    if n % 2 == 0:
                        P.op("act", lambda e, n=n, bk=bk: e.copy(out=qxT[:, n, :], in_=bank(bk)), reads=[bankB[bk]], writes=[QXT])
                    else:
                        P.op("dve", lambda e, n=n, bk=bk: e.tensor_copy(out=qxT[:, n, :], in_=bank(bk)), reads=[bankB[bk]], writes=[QXT])
                for hh in range(4):
                    ps_ = hh % 2
                    for mc in range(2):
                        bk = mmbank()

                        def f(e, hh=hh, mc=mc, bk=bk):
                            for dc in range(2):
                                ins = e.matmul(bank(bk), lhsT=kxT[:, 2 * hh + dc, mc * 128:(mc + 1) * 128], rhs=qxT[:, 2 * hh + dc, :],
                                               start=(dc == 0), stop=(dc == 1))
                            return ins
                        P.op("pe", f, reads=[KV, QXT], writes=[bankB[bk]])
                        P.op("act", lambda e, ps_=ps_, mc=mc, bk=bk: e.activation(out=pxT[ps_][:, mc, :], in_=bank(bk), func=AF.Exp, scale=1.0 / 16.0),
                             reads=[bankB[bk]], writes=[PXT[ps_]])
                    bk = mmbank()

                    def f(e, ps_=ps_, bk=bk):
                        for mc in range(2):
                            ins = e.matmul(bank(bk), lhsT=ones_bf, rhs=pxT[ps_][:, mc, :], start=(mc == 0), stop=(mc == 1))
                        return ins
                    P.op("pe", f, reads=[PXT[ps_], CONST], writes=[bankB[bk]])
                    P.op("act", lambda e, ps_=ps_, bk=bk: e.activation(out=rden[ps_], in_=bank(bk), func=AF.Ln), reads=[bankB[bk]], writes=[RDEN[ps_]])
                    P.op("act", lambda e, ps_=ps_: e.activation(out=rden[ps_], in_=rden[ps_], func=AF.Exp, scale=-1.0), reads=[RDEN[ps_]], writes=[RDEN[ps_]])
                    for dc in range(2):
                        bk = mmbank()

                        def f(e, hh=hh, dc=dc, ps_=ps_, bk=bk):
                            for mc in range(2):
                                ins = e.matmul(bank(bk), lhsT=vx[:, mc, (2 * hh + dc) * 128:(2 * hh + dc + 1) * 128], rhs=pxT[ps_][:, mc, :],
                                               start=(mc == 0), stop=(mc == 1))
                            return ins
                        P.op("pe", f, reads=[KV, PXT[ps_]], writes=[bankB[bk]])
                        P.op("dve", lambda e, hh=hh, dc=dc, ps_=ps_, bk=bk: e.tensor_tensor(out=oxT[:, 2 * hh + dc, :], in0=bank(bk), in1=rden[ps_], op=ALU.mult),
                             reads=[bankB[bk], RDEN[ps_]], writes=TT)
                for b4 in range(4):
                    b = half * 4 + b4
                    for nh in range(2):
                        bk = mmbank()

                        def f(e, b4=b4, nh=nh, bk=bk):
                            for fc in range(8):
                                ins = e.matmul(bank(bk), lhsT=oxT[:, fc, b4 * 128:(b4 + 1) * 128],
                                               rhs=w_xo_sb[:, fc, nh * 512:(nh + 1) * 512], start=(fc == 0), stop=(fc == 7))
                            return ins
                        P.op("pe", f, reads=[TT[b4], WXO], writes=[bankB[bk]])
                        P.op("dve", lambda e, b=b, nh=nh, bk=bk, hbuf=hbuf: e.tensor_tensor(out=hbuf[:, b, nh * 512:(nh + 1) * 512],
                                                                                 in0=hbuf[:, b, nh * 512:(nh + 1) * 512], in1=bank(bk), op=ALU.add),
                             reads=[bankB[bk], HBUF[b], HBUF2[b]], writes=[HBUF[b], HBUF2[b]])
            RL_BK = 7
            RK_BK = 6
            for b in range(G):
                rs, rsb = rms_scale(hbuf[:, b, 0:D], [HBUF[b], HBUF2[b]], D)
                P.op("dve", lambda e, b=b, rs=rs, hbuf=hbuf: e.scalar_tensor_tensor(out=n32, in0=hbuf[:, b, 0:D], scalar=rs, in1=g_f,
                                                                         op0=ALU.mult, op1=ALU.mult),
                     reads=[HBUF[b], HBUF2[b], rsb, CONST, GC], writes=[N32])
                t32 = bank(4, 2)

                def f(e):
                    for c in range(8):
                        ins = e.transpose(out=t32[:, c * 128:(c + 1) * 128], in_=n32[:, c * 128:(c + 1) * 128], identity=ident_f)
                    return ins
                P.op("pe", f, reads=[N32, CONST], writes=[bankB[4], bankB[5]])
                P.op("act", lambda e: e.copy(out=h32T, in_=t32.rearrange("p (c t) -> p c t", t=128)),
                     reads=[bankB[4], bankB[5]], writes=[H32T])

                def f(e, b=b):
                    for fc in range(8):
                        ins = e.matmul(bank(RL_BK)[:, b * 32:b * 32 + 20], lhsT=h32T[:, fc, :], rhs=wr_sb[:, fc, :],
                                       start=(fc == 0), stop=(fc == 7))
                    return ins
                P.op("pe", f, reads=[H32T, WR], writes=[bankB[RL_BK]])
            rl3 = bank(RL_BK)[:, 0:G * 32].rearrange("p (g n) -> p g n", n=32)
            V = lambda fn, reads, writes: P.op("dve", fn, reads=reads, writes=writes)
            V(lambda e: e.tensor_tensor(out=lg, in0=rl3[:, :, 0:20], in1=rb3, op=ALU.add), [bankB[RL_BK], CONST, GC], [RT])
            if debug:
                P.dma("sp", "dbg", dbg_lg[sc], lg.rearrange("p g n -> p (g n)"), reads=[RT])
            lgg = lg[:, :, 0:4]
            lge = lg[:, :, 4:20]
            bc = lambda ap, n: ap.rearrange("p (g o) -> p g o", o=1).to_broadcast([128, G, n])
            V(lambda e: e.tensor_reduce(out=r_mg, in_=lgg, axis=AX.X, op=ALU.max), [RT], [RT])
            V(lambda e: e.tensor_tensor(out=r_sh, in0=lgg, in1=bc(r_mg, 4), op=ALU.subtract), [RT], [RT])
            P.op("act", lambda e: e.activation(out=r_sh, in_=r_sh, func=AF.Exp), reads=[RT], writes=[RT])
            V(lambda e: e.tensor_reduce(out=r_gv, in_=r_sh, axis=AX.X, op=ALU.add), [RT], [RT])
            V(lambda e: e.reciprocal(out=r_gv, in_=r_gv), [RT], [RT])
            V(lambda e: e.tensor_tensor(out=r_oh, in0=lgg, in1=bc(r_mg, 4), op=ALU.is_equal), [RT], [RT])
            V(lambda e: e.tensor_tensor(out=r_oh, in0=r_oh, in1=w8421_3, op=ALU.mult), [RT, CONST], [RT])
            V(lambda e: e.tensor_reduce(out=r_m0, in_=r_oh, axis=AX.X, op=ALU.max), [RT], [RT])
            V(lambda e: e.tensor_tensor(out=r_ohk, in0=r_oh, in1=bc(r_m0, 4), op=ALU.is_equal), [RT], [RT])
            V(lambda e: e.tensor_copy(out=oh_bf, in_=r_ohk.rearrange("p g n -> p (g n)")), [RT], [RT])
            if debug:
                P.dma("sp", "dbg", dbg_oh[sc], r_ohk.rearrange("p g n -> p (g n)"), reads=[RT])
                P.dma("sp", "dbg", dbg_h[sc], hbuf.rearrange("p g n -> p (g n)"), reads=HBUF + HBUF2)
            V(lambda e: e.tensor_scalar(out=r_oh, in0=r_ohk, scalar1=-1.0, scalar2=BIG, op0=ALU.add, op1=ALU.mult), [RT], [RT])
            ml4 = r_ml.rearrange("p g (a b) -> p g a b", b=4)
            V(lambda e: e.tensor_tensor(out=ml4, in0=lge.rearrange("p g (a b) -> p g a b", b=4),
                                        in1=r_oh.rearrange("p g (a o) -> p g a o", o=1).to_broadcast([128, G, 4, 4]), op=ALU.add), [RT], [RT])
            V(lambda e: e.tensor_reduce(out=r_m1, in_=r_ml, axis=AX.X, op=ALU.max), [RT], [RT])
            V(lambda e: e.tensor_tensor(out=r_oh1, in0=r_ml, in1=bc(r_m1, 16), op=ALU.is_equal), [RT], [RT])
            V(lambda e: e.scalar_tensor_tensor(out=r_ml2, in0=r_oh1, scalar=-BIG, in1=r_ml, op0=ALU.mult, op1=ALU.add), [RT], [RT])
            V(lambda e: e.tensor_reduce(out=r_m2, in_=r_ml2, axis=AX.X, op=ALU.max), [RT], [RT])
            V(lambda e: e.tensor_tensor(out=r_oh2, in0=r_ml2, in1=bc(r_m2, 16), op=ALU.is_equal), [RT], [RT])
            V(lambda e: e.tensor_tensor(out=r_e, in0=r_m2, in1=r_m1, op=ALU.subtract), [RT], [RT])
            P.op("act", lambda e: e.activation(out=r_e, in_=r_e, func=AF.Exp), reads=[RT], writes=[RT])
            V(lambda e: e.tensor_scalar(out=r_w1, in0=r_e, scalar1=1.0, scalar2=0.0, op0=ALU.add, op1=ALU.add), [RT], [RT])
            V(lambda e: e.reciprocal(out=r_w1, in_=r_w1), [RT], [RT])
            V(lambda e: e.tensor_tensor(out=r_w2, in0=r_e, in1=r_w1, op=ALU.mult), [RT], [RT])
            V(lambda e: e.tensor_tensor(out=r_w1, in0=r_w1, in1=r_gv, op=ALU.mult), [RT], [RT])
            V(lambda e: e.tensor_tensor(out=r_w2, in0=r_w2, in1=r_gv, op=ALU.mult), [RT], [RT])
            V(lambda e: e.tensor_tensor(out=r_oh1, in0=r_oh1, in1=bc(r_w1, 16), op=ALU.mult), [RT], [RT])
            V(lambda e: e.tensor_tensor(out=r_oh2, in0=r_oh2, in1=bc(r_w2, 16), op=ALU.mult), [RT], [RT])
            V(lambda e: e.tensor_tensor(out=gate, in0=r_oh1, in1=r_oh2, op=ALU.add), [RT], [GATE])

            P.op("pe", lambda e: e.matmul(bank(RK_BK)[:, 0:G * 4], lhsT=m01_sb, rhs=oh_bf, start=True, stop=True),
                 reads=[RT, CONST], writes=[bankB[RK_BK]])
            P.op("pe", lambda e: e.matmul(bank(RK_BK)[:, 32:32 + G * 4], lhsT=ones_bf, rhs=oh_bf, start=True, stop=True),
                 reads=[RT, CONST], writes=[bankB[RK_BK]])
            rank3 = bank(RK_BK)[:, 0:G * 4].rearrange("p (g n) -> p g n", n=4)
            tot3_ = bank(RK_BK)[:, 32:32 + G * 4].rearrange("p (g n) -> p g n", n=4)
            V(lambda e: e.tensor_copy(out=basec[:, 0, :], in_=cnt_run), [CNT], [RT])
            for b in range(1, G):
                V(lambda e, b=b: e.tensor_tensor(out=basec[:, b, :], in0=basec[:, b - 1, :], in1=tot3_[:, b - 1, :], op=ALU.add),
                  [RT, bankB[RK_BK]], [RT])
            V(lambda e: e.tensor_tensor(out=cnt_run, in0=basec[:, G - 1, :], in1=tot3_[:, G - 1, :], op=ALU.add), [RT, bankB[RK_BK]], [CNT])
            V(lambda e: e.tensor_tensor(out=dtmp, in0=basec, in1=rank3, op=ALU.add), [RT, bankB[RK_BK]], [RT])
            V(lambda e: e.tensor_tensor(out=dtmp, in0=dtmp, in1=r_ohk, op=ALU.mult), [RT], [RT])
            V(lambda e: e.tensor_reduce(out=dest_f, in_=dtmp, axis=AX.X, op=ALU.add), [RT], [RT])
            di = dest_i[sc % 2]
            V(lambda e, di=di: e.tensor_copy(out=di, in_=dest_f), [RT], [DEST[sc % 2]])
            V(lambda e, hbuf=hbuf: e.tensor_reduce(out=hbuf[:, :, 1024:1028], in_=gate.rearrange("p b (g n) -> p b n g", n=4), axis=AX.X, op=ALU.add),
              [GATE], HBUF2)
            V(lambda e, hbuf=hbuf, sc=sc: e.tensor_copy(out=hbuf[:, :, 1028:1029], in_=iota_f[:, sc * G:(sc + 1) * G].rearrange("p (g o) -> p g o", o=1)),
              [CONST2], HBUF2)
            for b in range(G):
                P.dma_fn("pool", f"sct{b % 4}", lambda e, b=b, hbuf=hbuf, di=di: e.indirect_dma_start(
                    out=sorted_d[:, :], out_offset=bass.IndirectOffsetOnAxis(ap=di[:, b:b + 1], axis=0),
                    in_=hbuf[:, b, :], in_offset=None, bounds_check=P.regs["bc_sorted"], oob_is_err=False),
                    reads=[HBUF[b], HBUF2[b], DEST[sc % 2]])

        pad_f = A.alloc(16, F32)
        pad_i = A.alloc(16, F32).bitcast(I32)
        ng_f = A.alloc(4, F32)
        PADB = Buf("pad")
        FLG3 = Buf("flags3")
        for g in range(4):
            P.op("dve", lambda e, g=g: e.tensor_scalar(out=pad_f[:, g * 4:(g + 1) * 4], in0=iota_f[:, 0:4], scalar1=cnt_run[:, g:g + 1], scalar2=0.0,
                                                       op0=ALU.add, op1=ALU.add), reads=[CNT, CONST2], writes=[PADB])
        P.op("dve", lambda e: e.tensor_copy(out=pad_i, in_=pad_f), reads=[PADB], writes=[PADB])
        for i_ in range(16):
            P.dma_fn("pool", f"sct{i_ % 4}", lambda e, i_=i_: e.indirect_dma_start(
                out=sorted_d[:, :], out_offset=bass.IndirectOffsetOnAxis(ap=pad_i[:, i_:i_ + 1], axis=0),
                in_=padrow, in_offset=None, bounds_check=P.regs["bc_sorted"], oob_is_err=False), reads=[PADB, CONST])
        P.op("dve", lambda e: e.tensor_tensor(out=ng_f, in0=cnt_run, in1=goff, op=ALU.subtract), reads=[CNT, CONST2], writes=[PADB])
        for g in range(4):
            P.op("dve", lambda e, g=g: e.tensor_scalar(out=flags3[:, g * NT3:(g + 1) * NT3], in0=thr512, scalar1=ng_f[:, g:g + 1], scalar2=0.0,
                                                       op0=ALU.is_lt, op1=ALU.add), reads=[PADB, CONST2], writes=[FLG3])

        P.barrier()
        A.release(mark0)

        EW = 8 * 512 + 2 * D
        wgrp = [A.alloc(4 * EW) for _ in range(2)]
        WGRP = [Buf(f"wgrp{i}") for i in range(2)]
        hb2 = [A.alloc(4 * ROWW, F32).rearrange("p (b d) -> p b d", d=ROWW) for _ in range(2)]
        HB2 = [[Buf(f"hb2_{i}_{b}") for b in range(4)] for i in range(2)]
        HB2R = [[Buf(f"hb2r_{i}_{b}") for b in range(4)] for i in range(2)]
        HB2T = [Buf(f"hb2t_{i}") for i in range(2)]
        h3Ts = [A.alloc(8 * 512).rearrange("p (c t) -> p c t", t=512) for _ in range(2)]
        H3Ts = [[Buf(f"h3T{i}_{b}") for b in range(4)] for i in range(2)]
        nb3 = [A.alloc(D) for _ in range(2)]
        NBB3 = [Buf(f"nbb{i}") for i in range(2)]
        o_st = [A.alloc(D, F32) for _ in range(2)]
        OST = [Buf(f"ost{i}") for i in range(2)]
        tmpb = [A.alloc(512, F32) for _ in range(4)]
        TMPB = [Buf(f"tmpb{i}") for i in range(4)]
        sg = [A.alloc(512, F32) for _ in range(2)]
        SG = [Buf(f"sg{i}") for i in range(2)]
        hid = [A.alloc(2 * 512).rearrange("p (j t) -> p j t", t=512) for _ in range(2)]
        HID = [[Buf(f"hid{i}_{j}") for j in range(2)] for i in range(2)]
        idx_i = [A.alloc(4, F32).bitcast(I32) for _ in range(2)]
        IDX = [Buf(f"idx{i}") for i in range(2)]

        def load_group(g):
            sl = g % 2
            for el in range(4):
                e_ = g * 4 + el
                gu = wgrp[sl][:, el * EW:el * EW + 8 * 512].rearrange("p (c n) -> p c n", n=512)
                dn = wgrp[sl][:, el * EW + 8 * 512:(el + 1) * EW].rearrange("p (j n) -> p j n", n=D)
                P.dma("sp", f"weg{sl}", gu[:, :, 0:256], wg_b[e_].rearrange("(c p) n -> p c n", p=128), reads=[WGB], writes=[WGRP[sl]])
                P.dma("sp", f"weu{sl}", gu[:, :, 256:512], wu_b[e_].rearrange("(c p) n -> p c n", p=128), reads=[WUB], writes=[WGRP[sl]])
                P.dma("sp", f"wed{sl}", dn, wd_b[e_].rearrange("(j p) n -> p j n", p=128), reads=[WDB], writes=[WGRP[sl]])

        tiles3 = [(g, k) for g in range(4) for k in range(NT3)]

        def flag_of(ti):
            g, k = tiles3[ti]
            return flags3[0:1, g * NT3 + k:g * NT3 + k + 1]

        def load_tile(ti):
            g, k = tiles3[ti]
            s = ti % 2
            r0 = g * CAPG + k * 512
            P.region_begin(("pool",), flag_of(ti), [FLG3])
            P.dma("pool", f"srt{s}", hb2[s], sorted_d[r0:r0 + 512, :].rearrange("(b p) d -> p b d", p=128),
                  writes=HB2[s] + HB2R[s] + [HB2T[s]])
            P.region_end()

        ebk = [0]
        tmpc = [0]

        def mmbank6():
            b_ = 2 + ebk[0] % 6
            ebk[0] += 1
            return b_

        def pro_norm(ti, b):
            s = ti % 2
            hb = hb2[s]
            HB_, HBR_ = HB2[s], HB2R[s]
            k2 = b % 2
            if b == 3:
                P.op("dve", lambda e: e.tensor_copy(out=idx_i[s].rearrange("p (b o) -> p b o", o=1), in_=hb[:, :, 1028:1029]),
                     reads=[HB2T[s]], writes=[IDX[s]])
            rs, rsb = rms_scale(hb[:, b, 0:D], [HB_[b], HBR_[b]], D)
            P.op("dve", lambda e, b=b, k2=k2, rs=rs: e.scalar_tensor_tensor(out=nb3[k2], in0=hb[:, b, 0:D], scalar=rs, in1=g_f,
                                                                           op0=ALU.mult, op1=ALU.mult),
                 reads=[HB_[b], HBR_[b], rsb, CONST, GC], writes=[NBB3[k2]])

        def pro_transp(ti, b):
            s = ti % 2
            k2 = b % 2
            transposes_bf(nb3[k2], [NBB3[k2]], b % 2, h3Ts[s][:, :, b * 128:(b + 1) * 128], [H3Ts[s][b]],
                          copy_eng=("act" if b % 2 == 0 else "dve"))

        def prologue(ti):
            P.region_begin(("pe", "act", "dve", "pool"), flag_of(ti), [FLG3])
            for b in range(4):
                pro_norm(ti, b)
                pro_transp(ti, b)
            P.region_end()

        def experts(ti, els, with_epilogue, nxt=None):
            g, k = tiles3[ti]
            s = ti % 2
            hb = hb2[s]
            HB_, HBR_ = HB2[s], HB2R[s]
            h3T, H3T = h3Ts[s], H3Ts[s]
            hs = ti % 2
            P.region_begin(("pe", "act", "dve", "pool"), flag_of(ti), [FLG3])
            for el in els:
                sl = g % 2
                if nxt is not None:
                    pro_norm(nxt, el)
                gu = wgrp[sl][:, el * EW:el * EW + 8 * 512].rearrange("p (c n) -> p c n", n=512)
                dn = wgrp[sl][:, el * EW + 8 * 512:(el + 1) * EW].rearrange("p (j n) -> p j n", n=D)
                for jj in range(2):
                    bkg = mmbank6()

                    def f(e, jj=jj, bkg=bkg, gu=gu):
                        for fc in range(8):
                            ins = e.matmul(bank(bkg), lhsT=gu[:, fc, jj * 128:(jj + 1) * 128], rhs=h3T[:, fc, :],
                                           start=(fc == 0), stop=(fc == 7))
                        return ins
                    P.op("pe", f, reads=H3T + [WGRP[sl]], writes=[bankB[bkg]])
                    bku = mmbank6()

                    def f(e, jj=jj, bku=bku, gu=gu):
                        for fc in range(8):
                            ins = e.matmul(bank(bku), lhsT=gu[:, fc, 256 + jj * 128:256 + (jj + 1) * 128], rhs=h3T[:, fc, :],
                                           start=(fc == 0), stop=(fc == 7))
                        return ins
                    P.op("pe", f, reads=H3T + [WGRP[sl]], writes=[bankB[bku]])
                    P.op("act", lambda e, jj=jj, bkg=bkg: e.activation(out=sg[jj], in_=bank(bkg), func=AF.Silu),
                         reads=[bankB[bkg]], writes=[SG[jj]])
                    P.op("dve", lambda e, jj=jj, bku=bku: e.tensor_tensor(out=hid[hs][:, jj, :], in0=sg[jj], in1=bank(bku), op=ALU.mult),
                         reads=[SG[jj], bankB[bku]], writes=[HID[hs][jj]])
                for b in range(4):
                    gcol = hb[:, b, 1024 + el:1025 + el]
                    for nh in range(2):
                        bk = mmbank6()

                        def f(e, b=b, nh=nh, bk=bk, dn=dn):
                            for jj in range(2):
                                ins = e.matmul(bank(bk), lhsT=hid[hs][:, jj, b * 128:(b + 1) * 128], rhs=dn[:, jj, nh * 512:(nh + 1) * 512],
                                               start=(jj == 0), stop=(jj == 1))
                            return ins
                        P.op("pe", f, reads=HID[hs] + [WGRP[sl]], writes=[bankB[bk]])
                        if nh == 0:
                            P.op("dve", lambda e, b=b, bk=bk, gcol=gcol: e.scalar_tensor_tensor(
                                out=hb[:, b, 0:512], in0=bank(bk), scalar=gcol, in1=hb[:, b, 0:512], op0=ALU.mult, op1=ALU.add),
                                reads=[bankB[bk], HB_[b], HB2T[s]], writes=[HB_[b]])
                        else:
                            tk = tmpc[0] % 4
                            tmpc[0] += 1
                            P.op("act", lambda e, bk=bk, gcol=gcol, tk=tk: e.activation(out=tmpb[tk], in_=bank(bk), func=AF.Copy, scale=gcol),
                                 reads=[bankB[bk], HB2T[s]], writes=[TMPB[tk]])
                            P.op("pool", lambda e, b=b, tk=tk: e.tensor_tensor(out=hb[:, b, 512:1024], in0=hb[:, b, 512:1024], in1=tmpb[tk], op=ALU.add),
                                 reads=[TMPB[tk], HBR_[b]], writes=[HBR_[b]])
                if nxt is not None:
                    pro_transp(nxt, el)
            if with_epilogue:
                for b in range(4):
                    k2 = b % 2
                    rs, rsb = rms_scale(hb[:, b, 0:D], [HB_[b], HBR_[b]], D)
                    P.op("dve", lambda e, b=b, k2=k2, rs=rs: e.scalar_tensor_tensor(out=o_st[k2], in0=hb[:, b, 0:D], scalar=rs, in1=g_fin,
                                                                                   op0=ALU.mult, op1=ALU.mult),
                         reads=[HB_[b], HBR_[b], rsb, CONST, GC], writes=[OST[k2]])
                    P.dma_fn("pool", f"osc{k2}", lambda e, b=b, k2=k2: e.indirect_dma_start(
                        out=out_d[:, :], out_offset=bass.IndirectOffsetOnAxis(ap=idx_i[s][:, b:b + 1], axis=0),
                        in_=o_st[k2], in_offset=None, bounds_check=P.regs["bc_out"], oob_is_err=False),
                        reads=[OST[k2], IDX[s]])
            P.region_end()

        load_group(0)
        load_tile(0)
        for ti in range(len(tiles3)):
            g, k = tiles3[ti]
            if k == 0 and g + 1 < 4:
                load_group(g + 1)
            if ti + 1 < len(tiles3):
                load_tile(ti + 1)
            if k == 0:
                prologue(ti)
            experts(ti, [0, 1, 2, 3], True, nxt=(ti + 1 if k + 1 < NT3 else None))

        P.barrier()
        P.emit()
    return nc


_CACHE = {}


def _get_prog(S, debug=False):
    key = (S, debug)
    if key not in _CACHE:
        _CACHE[key] = build_program(S, debug)
    return _CACHE[key]


WNAMES = ["norm_mix", "w_in", "b_forget", "norm_fox_out", "norm_sb_out", "w_out", "norm_xattn", "norm_mem",
          "w_xq", "w_xkv", "w_xo", "norm_ffn", "w_router_group", "b_router_group", "w_router_expert",
          "b_router_expert", "w_exp_gate", "w_exp_up", "w_exp_down"]


def make_in_maps(inputs):
    x = np.asarray(inputs["x"], dtype=np.float32)
    mem = np.asarray(inputs["mem"], dtype=np.float32)
    B = x.shape[0]
    shared = {}
    for k in WNAMES:
        a = np.asarray(inputs[k], dtype=np.float32)
        shared[k] = np.ascontiguousarray(a[0])
    shared["norm_final"] = np.ascontiguousarray(np.asarray(inputs["norm_final"], dtype=np.float32))
    maps = []
    for b in range(B):
        m = dict(shared)
        m["x"] = np.ascontiguousarray(x[b])
        m["mem"] = np.ascontiguousarray(mem[b])
        maps.append(m)
    return maps


def kernel(**inputs):
    x = inputs["x"]
    B, S, _ = x.shape
    nc = _get_prog(S)
    maps = make_in_maps(inputs)
    res = run_bass_kernel_spmd(nc, maps, core_ids=list(range(B)))
    out = np.stack([np.asarray(r["out"], dtype=np.float32) for r in res.results], axis=0)
    return out
```
